# Optimizing a Trainium2 kernel written in Bass

```python
import math
import jax, jax.numpy as jnp
from jax import lax
import numpy as np

D_MODEL = 2048
BATCH = 2
SEQ = 8192
DEPTH = 4
DEC_BATCH = 4
DEC_SEQ = 8192
PAST_LEN = 128

N_MIXERS = 3
HEAD_DIM = 128
ROPE_THETA = 500000.0
ROT_FRAC = 4
NORM_EPS = 1e-6
Q_BLOCK = 128
NEG = -1e30

A_COMP = HEAD_DIM
A_VDIM = 2 * HEAD_DIM
A_HEADS = D_MODEL // A_VDIM
A_WIDTH = A_HEADS * A_VDIM

B_HEADS = D_MODEL // HEAD_DIM
B_KV_HEADS = B_HEADS // 4
B_WINDOW = 128

C_CONFIGS = ((128, 1), (512, 4), (2048, 16))
C_GROUPS = len(C_CONFIGS)
C_HEADS = D_MODEL // HEAD_DIM // 2
C_WIDTH = C_HEADS * HEAD_DIM

FFN_DIM = 2 * D_MODEL
MOE_DIM = D_MODEL // 2
N_EXPERTS = 8
TOP_K = 2

N_A = (DEPTH + 2) // 3
N_B = (DEPTH + 1) // 3
N_C = DEPTH // 3
N_DENSE = (DEPTH + 1) // 2
N_MOE = DEPTH // 2

kernel_name = 'hybrid_bidir_encoder_adaln'


def rmsnorm(x, g):
    xf = x.astype(jnp.float32)
    y = xf * lax.rsqrt(jnp.mean(xf * xf, axis=-1, keepdims=True) + NORM_EPS)
    return y.astype(x.dtype) * g


def rope(x, pos, rot):
    half = rot // 2
    inv = ROPE_THETA ** (-jnp.arange(half, dtype=jnp.float32) * (2.0 / rot))
    ang = pos[:, None] * inv[None, :]
    cos = jnp.cos(ang)[None, :, None, :]
    sin = jnp.sin(ang)[None, :, None, :]
    x1 = x[..., :half].astype(jnp.float32)
    x2 = x[..., half:rot].astype(jnp.float32)
    xr = jnp.concatenate([x1 * cos - x2 * sin, x2 * cos + x1 * sin], axis=-1).astype(x.dtype)
    return jnp.concatenate([xr, x[..., rot:]], axis=-1)


def lambda_init(layer):
    return 0.8 - 0.6 * math.exp(-0.3 * layer)


def banded_attention(q, k, v, half, sink=None):
    N, L, Hkv, G, Dh = q.shape
    blk = half
    nb = -(-L // blk)
    Lp = nb * blk
    q = jnp.pad(q, ((0, 0), (0, Lp - L), (0, 0), (0, 0), (0, 0)))
    kv_pad = ((0, 0), (blk, Lp - L + blk), (0, 0), (0, 0))
    kp = jnp.pad(k, kv_pad).reshape(N, nb + 2, blk, Hkv, Dh)
    vp = jnp.pad(v, kv_pad).reshape(N, nb + 2, blk, Hkv, Dh)
    kw = jnp.concatenate([kp[:, :-2], kp[:, 1:-1], kp[:, 2:]], axis=2)
    vw = jnp.concatenate([vp[:, :-2], vp[:, 1:-1], vp[:, 2:]], axis=2)
    qb = q.reshape(N, nb, blk, Hkv, G, Dh)
    s = jnp.einsum('nbqhgd,nbjhd->nbhgqj', qb, kw,
                   preferred_element_type=jnp.float32) * (Dh ** -0.5)
    nbi = jnp.arange(nb)[:, None, None]
    qpos = nbi * blk + jnp.arange(blk)[None, :, None]
    kpos = (nbi - 1) * blk + jnp.arange(3 * blk)[None, None, :]
    valid = (jnp.abs(kpos - qpos) <= half) & (kpos >= 0) & (kpos < L)
    s = jnp.where(valid[None, :, None, None], s, NEG)
    m = s.max(axis=-1)
    if sink is not None:
        m = jnp.maximum(m, sink[:, :, None])
    e = jnp.exp(s - m[..., None])
    den = e.sum(axis=-1)
    if sink is not None:
        den = den + jnp.exp(sink[:, :, None] - m)
    o = jnp.einsum('nbhgqj,nbjhd->nbqhgd', (e / den[..., None]).astype(v.dtype), vw)
    lse = m + jnp.log(den)
    o = o.reshape(N, Lp, Hkv, G, Dh)[:, :L]
    lse = lse.transpose(0, 1, 4, 2, 3).reshape(N, Lp, Hkv, G)[:, :L]
    return o, lse


def diff_attention(q, k, v, lam):
    B, S, H, _, Dc = q.shape
    nb = S // Q_BLOCK
    qb = q.reshape(B, nb, Q_BLOCK, H, 2, Dc).transpose(1, 0, 2, 3, 4, 5)

    def block(qi):
        s = jnp.einsum('bqhcd,bkhcd->bhcqk', qi, k,
                       preferred_element_type=jnp.float32) * (Dc ** -0.5)
        p = jax.nn.softmax(s, axis=-1)
        a = p[:, :, 0] - lam * p[:, :, 1]
        return jnp.einsum('bhqk,bkhd->bqhd', a.astype(v.dtype), v)

    o = lax.map(block, qb)
    return o.transpose(1, 0, 2, 3, 4).reshape(B, S, H, v.shape[-1])


def mixer_a(h, w_in, w_out, lam_p, subln, lam0, pos):
    B, S, _ = h.shape
    qkv = h @ w_in
    q = qkv[..., :A_WIDTH].reshape(B, S, A_HEADS * 2, A_COMP)
    k = qkv[..., A_WIDTH:2 * A_WIDTH].reshape(B, S, A_HEADS * 2, A_COMP)
    v = qkv[..., 2 * A_WIDTH:].reshape(B, S, A_HEADS, A_VDIM)
    rot = A_COMP // ROT_FRAC
    q = rope(q, pos, rot).reshape(B, S, A_HEADS, 2, A_COMP)
    k = rope(k, pos, rot).reshape(B, S, A_HEADS, 2, A_COMP)
    lp = lam_p.astype(jnp.float32)
    lam = jnp.exp(jnp.sum(lp[0] * lp[1])) - jnp.exp(jnp.sum(lp[2] * lp[3])) + lam0
    o = diff_attention(q, k, v, lam)
    o = rmsnorm(o, subln) * (1.0 - lam0)
    return o.reshape(B, S, A_WIDTH) @ w_out


def mixer_b(h, w_in, w_out, sink, pos):
    B, S, _ = h.shape
    qkv = h @ w_in
    nq = B_HEADS * HEAD_DIM
    nk = B_KV_HEADS * HEAD_DIM
    q = qkv[..., :nq].reshape(B, S, B_HEADS, HEAD_DIM)
    k = qkv[..., nq:nq + nk].reshape(B, S, B_KV_HEADS, HEAD_DIM)
    v = qkv[..., nq + nk:].reshape(B, S, B_KV_HEADS, HEAD_DIM)
    rot = HEAD_DIM // ROT_FRAC
    q = rope(q, pos, rot).reshape(B, S, B_KV_HEADS, B_HEADS // B_KV_HEADS, HEAD_DIM)
    k = rope(k, pos, rot)
    o, _ = banded_attention(q, k, v, B_WINDOW,
                            sink.astype(jnp.float32).reshape(B_KV_HEADS, B_HEADS // B_KV_HEADS))
    return o.reshape(B, S, nq) @ w_out


def dilated_attention(q, k, v, window, dil):
    B, S, H, Dh = q.shape
    L = S // dil

    def to_sub(t):
        return t.reshape(B, L, dil, H, Dh).swapaxes(1, 2).reshape(B * dil, L, H, Dh)

    o, lse = banded_attention(to_sub(q)[:, :, :, None, :], to_sub(k), to_sub(v),
                              window // (2 * dil))
    o = o[:, :, :, 0].reshape(B, dil, L, H, Dh).swapaxes(1, 2).reshape(B, S, H, Dh)
    lse = lse[..., 0].reshape(B, dil, L, H).swapaxes(1, 2).reshape(B, S, H)
    return o, lse


def mixer_c(h, w_in, w_out, pos):
    B, S, _ = h.shape
    qkv = (h @ w_in).reshape(B, S, 3, C_GROUPS, C_HEADS, HEAD_DIM)
    rot = HEAD_DIM // ROT_FRAC
    q = rope(qkv[:, :, 0].reshape(B, S, C_GROUPS * C_HEADS, HEAD_DIM), pos, rot)
    k = rope(qkv[:, :, 1].reshape(B, S, C_GROUPS * C_HEADS, HEAD_DIM), pos, rot)
    q = q.reshape(B, S, C_GROUPS, C_HEADS, HEAD_DIM)
    k = k.reshape(B, S, C_GROUPS, C_HEADS, HEAD_DIM)
    v = qkv[:, :, 2]
    outs, lses = [], []
    for g, (window, dil) in enumerate(C_CONFIGS):
        o_g, lse_g = dilated_attention(q[:, :, g], k[:, :, g], v[:, :, g], window, dil)
        outs.append(o_g)
        lses.append(lse_g)
    o = jnp.stack(outs)
    alpha = jax.nn.softmax(jnp.stack(lses), axis=0)
    o = jnp.sum(alpha[..., None].astype(o.dtype) * o, axis=0)
    return o.reshape(B, S, C_WIDTH) @ w_out


def swiglu(h, w_gu, w_down):
    f = w_down.shape[0]
    gu = h @ w_gu
    return (jax.nn.silu(gu[..., :f]) * gu[..., f:]) @ w_down


def moe_swiglu(h, router, w_gu, w_down):
    logits = jnp.einsum('bsd,de->bse', h, router, preferred_element_type=jnp.float32)
    top_v, top_i = lax.top_k(logits, TOP_K)
    top_w = jax.nn.softmax(top_v, axis=-1)
    gates = jnp.sum(jax.nn.one_hot(top_i, N_EXPERTS, dtype=jnp.float32) * top_w[..., None],
                    axis=-2)
    y = jnp.zeros_like(h)
    for e in range(N_EXPERTS):
        y = y + gates[..., e:e + 1].astype(h.dtype) * swiglu(h, w_gu[e], w_down[e])
    return y


def trunk(x, c, ada_w, ada_b, norm_mix, norm_ffn, a_w_in, a_w_out, a_lambda, a_subln,
          b_w_in, b_w_out, b_sink, c_w_in, c_w_out, f_w_gu, f_w_down,
          moe_router, moe_w_gu, moe_w_down, final_norm):
    B, S, _ = x.shape
    pos = jnp.arange(S, dtype=jnp.float32)
    cs = jax.nn.silu(c)
    for i in range(DEPTH):
        mod = cs @ ada_w[i] + ada_b[i]
        sh_m, sc_m, g_m, sh_f, sc_f, g_f = [t[:, None, :] for t in jnp.split(mod, 6, axis=-1)]
        h = rmsnorm(x, norm_mix[i]) * (1 + sc_m) + sh_m
        kind = i % N_MIXERS
        j = i // N_MIXERS
        if kind == 0:
            mix = mixer_a(h, a_w_in[j], a_w_out[j], a_lambda[j], a_subln[j], lambda_init(i), pos)
        elif kind == 1:
            mix = mixer_b(h, b_w_in[j], b_w_out[j], b_sink[j], pos)
        else:
            mix = mixer_c(h, c_w_in[j], c_w_out[j], pos)
        x = x + g_m * mix
        h = rmsnorm(x, norm_ffn[i]) * (1 + sc_f) + sh_f
        if i % 2 == 0:
            f = swiglu(h, f_w_gu[i // 2], f_w_down[i // 2])
        else:
            f = moe_swiglu(h, moe_router[i // 2], moe_w_gu[i // 2], moe_w_down[i // 2])
        x = x + g_f * f
    return rmsnorm(x, final_norm)


def setup_inputs(seed: int = 0) -> dict:
    key = jax.random.key(seed)
    ks = jax.random.split(key, 24)

    def nrm(k, shape, scale=1.0):
        return jax.random.normal(k, shape, jnp.float32) * scale

    D = D_MODEL
    sd = D ** -0.5
    return {
        'x_prompt': nrm(ks[0], (BATCH, SEQ, D)),
        'x_sample': nrm(ks[1], (DEC_BATCH, DEC_SEQ, D)),
        'c_prompt': nrm(ks[2], (BATCH, D)),
        'c_sample': nrm(ks[3], (DEC_BATCH, D)),
        'ada_w': nrm(ks[4], (DEPTH, D, 6 * D), 0.5 * sd),
        'ada_b': nrm(ks[5], (DEPTH, 6 * D), 0.02),
        'norm_mix': 1.0 + nrm(ks[6], (DEPTH, D), 0.02),
        'norm_ffn': 1.0 + nrm(ks[7], (DEPTH, D), 0.02),
        'a_w_in': nrm(ks[8], (N_A, D, 3 * A_WIDTH), sd),
        'a_w_out': nrm(ks[9], (N_A, A_WIDTH, D), A_WIDTH ** -0.5),
        'a_lambda': nrm(ks[10], (N_A, 4, A_COMP), 0.1),
        'a_subln': 1.0 + nrm(ks[11], (N_A, A_VDIM), 0.02),
        'b_w_in': nrm(ks[12], (N_B, D, (B_HEADS + 2 * B_KV_HEADS) * HEAD_DIM), sd),
        'b_w_out': nrm(ks[13], (N_B, B_HEADS * HEAD_DIM, D), (B_HEADS * HEAD_DIM) ** -0.5),
        'b_sink': nrm(ks[14], (N_B, B_HEADS), 0.5),
        'c_w_in': nrm(ks[15], (N_C, D, 3 * C_GROUPS * C_WIDTH), sd),
        'c_w_out': nrm(ks[16], (N_C, C_WIDTH, D), C_WIDTH ** -0.5),
        'f_w_gu': nrm(ks[17], (N_DENSE, D, 2 * FFN_DIM), sd),
        'f_w_down': nrm(ks[18], (N_DENSE, FFN_DIM, D), FFN_DIM ** -0.5),
        'moe_router': nrm(ks[19], (N_MOE, D, N_EXPERTS), sd),
        'moe_w_gu': nrm(ks[20], (N_MOE, N_EXPERTS, D, 2 * MOE_DIM), sd),
        'moe_w_down': nrm(ks[21], (N_MOE, N_EXPERTS, MOE_DIM, D), MOE_DIM ** -0.5),
        'final_norm': 1.0 + nrm(ks[22], (D,), 0.02),
    }


def reference(x_prompt, x_sample, c_prompt, c_sample, ada_w, ada_b, norm_mix, norm_ffn,
              a_w_in, a_w_out, a_lambda, a_subln, b_w_in, b_w_out, b_sink, c_w_in, c_w_out,
              f_w_gu, f_w_down, moe_router, moe_w_gu, moe_w_down, final_norm):
    y_prompt = trunk(x_prompt, c_prompt, ada_w, ada_b, norm_mix, norm_ffn, a_w_in, a_w_out,
                     a_lambda, a_subln, b_w_in, b_w_out, b_sink, c_w_in, c_w_out,
                     f_w_gu, f_w_down, moe_router, moe_w_gu, moe_w_down, final_norm)
    y_sample = trunk(x_sample, c_sample, ada_w, ada_b, norm_mix, norm_ffn, a_w_in, a_w_out,
                     a_lambda, a_subln, b_w_in, b_w_out, b_sink, c_w_in, c_w_out,
                     f_w_gu, f_w_down, moe_router, moe_w_gu, moe_w_down, final_norm)
    return (y_prompt, y_sample)
```

```python
import math
from contextlib import ExitStack

import numpy as np
import ml_dtypes

import concourse.bass as bass
import concourse.mybir as mybir
from concourse.bass_utils import run_bass_kernel_spmd

F32 = mybir.dt.float32
BF16 = mybir.dt.bfloat16
AF = mybir.ActivationFunctionType
ALU = mybir.AluOpType
AX = mybir.AxisListType

D = 2048
EPS = 1e-6
ENGS = ("pe", "act", "dve", "pool", "sp")
ENGATTR = {"pe": "tensor", "act": "scalar", "dve": "vector", "pool": "gpsimd", "sp": "sync"}


class Op:
    __slots__ = ("eng", "fn", "deps", "dwaits", "signal", "seq", "dsem", "is_dma")

    def __init__(self, eng, fn):
        self.eng = eng
        self.fn = fn
        self.deps = []
        self.dwaits = []
        self.signal = False
        self.seq = 0
        self.dsem = None
        self.is_dma = False


class DSem:
    __slots__ = ("h", "total")

    def __init__(self, h):
        self.h = h
        self.total = 0


class Buf:
    def __init__(self, name, t=None):
        self.name = name
        self.t = t
        self.w = {}
        self.r = {}
        self.wsem = None
        self.rsem = None
        self.excl = False

    def __getitem__(self, k):
        return self.t[k]


class _Recorder:
    def __getattr__(self, name):
        def f(*a, **kw):
            self.call = (name, a, kw)
        return f


class Prog:
    def __init__(self, nc, stack):
        self.nc = nc
        self.stack = stack
        self.ops = {e: [] for e in ENGS}
        self.esem = {e: stack.enter_context(nc.semaphore("es_" + e)) for e in ENGS}
        self.ecount = {e: 0 for e in ENGS}
        self.waited = {e: {} for e in ENGS}
        self.last_real = {e: None for e in ENGS}
        self.dsems = []
        self.free_dsems = []
        self.bufs = []
        self.ninstr = {e: 0 for e in ENGS}
        self.nwait = {e: 0 for e in ENGS}

    def reg(self, b):
        self.bufs.append(b)
        return b

    def new_dsem(self, name):
        if self.free_dsems:
            return self.free_dsems.pop()
        s = DSem(self.stack.enter_context(self.nc.semaphore("d%d" % len(self.dsems))))
        self.dsems.append(s)
        return s

    def release(self, bufs):
        for b in bufs:
            for s in (b.wsem, b.rsem):
                if s is not None:
                    self.free_dsems.append(s)
            b.wsem = b.rsem = None
            if b in self.bufs:
                self.bufs.remove(b)

    def _dep_on(self, op, prod, raw):
        if prod is op:
            return
        if prod.is_dma:
            op.dwaits.append((prod.dsem, prod.dsem.total))
            return
        if prod.eng == op.eng and not op.is_dma:
            if op.eng == "pe" or not raw:
                return
        prod.signal = True
        op.deps.append(prod)

    def _track(self, op, reads, writes):
        for b in reads:
            for p in b.w.values():
                self._dep_on(op, p, True)
            if b.excl:
                for p in b.r.values():
                    if p.eng != op.eng:
                        self._dep_on(op, p, False)
        for b in writes:
            for p in b.w.values():
                self._dep_on(op, p, False)
            for p in b.r.values():
                self._dep_on(op, p, False)
        key = ("d", id(op.dsem)) if op.is_dma else op.eng
        for b in writes:
            b.w[key] = op
        for b in reads:
            b.r[key] = op

    def op(self, eng, fn, reads=(), writes=()):
        rec = _Recorder()
        fn(rec)
        name, a, kw = rec.call
        o = Op(eng, lambda e, name=name, a=a, kw=kw: getattr(e, name)(*a, **kw))
        self._track(o, reads, writes)
        self.ops[eng].append(o)
        self.last_real[eng] = o
        return o

    def dma(self, out, in_, reads=(), writes=(), owner=None, load=True, q="sp", **kw):
        o = Op(q, None)
        o.is_dma = True
        if load:
            if owner.wsem is None:
                owner.wsem = self.new_dsem("w")
            o.dsem = owner.wsem
        else:
            if owner.rsem is None:
                owner.rsem = self.new_dsem("r")
            o.dsem = owner.rsem
        self._track(o, reads, writes)
        o.dsem.total += 16
        o.fn = lambda e: e.dma_start(out=out, in_=in_, **kw)
        self.ops[q].append(o)
        return o

    def barrier(self):
        lasts = [self.last_real[e] for e in ENGS if self.last_real[e] is not None]
        for e in ENGS:
            o = Op(e, None)
            for p in lasts:
                if p.eng != e:
                    p.signal = True
                    o.deps.append(p)
            for s in self.dsems:
                if s.total:
                    o.dwaits.append((s, s.total))
            self.ops[e].append(o)
        for b in self.bufs:
            b.w.clear()
            b.r.clear()
        self.last_real = {e: None for e in ENGS}

    def emit(self):
        nc = self.nc
        for e in ENGS:
            n = self.ecount[e]
            for o in self.ops[e]:
                if o.signal:
                    n += 1
                    o.seq = n
            self.ecount[e] = n
        with nc.Block() as block:
            for e in ENGS:
                ops = self.ops[e]

                def body(eng, ops=ops, e=e):
                    esem = self.esem
                    waited = self.waited[e]
                    ni = nw = 0
                    for o in ops:
                        need = {}
                        for p in o.deps:
                            k = ("e", p.eng)
                            if need.get(k, (None, 0))[1] < p.seq:
                                need[k] = (esem[p.eng], p.seq)
                        for s, v in o.dwaits:
                            k = ("d", id(s))
                            if need.get(k, (None, 0))[1] < v:
                                need[k] = (s.h, v)
                        for k, (h, v) in need.items():
                            if waited.get(k, 0) < v:
                                eng.wait_ge(h, v)
                                waited[k] = v
                                nw += 1
                        if o.fn is None:
                            continue
                        ins = o.fn(eng)
                        ni += 1
                        if o.is_dma:
                            ins.then_inc(o.dsem.h, 16)
                        elif o.signal:
                            ins.then_inc(esem[e], 1)
                    self.ninstr[e] += ni
                    self.nwait[e] += nw

                getattr(block, ENGATTR[e])(body)
        self.ops = {e: [] for e in ENGS}


class K:
    pass


class Phase:
    def __init__(self, k, name):
        self.k = k
        self.name = name
        self.stack = ExitStack()
        self.bufs = []
        self.n = 0

    def sb(self, shape, dt, name=None):
        self.n += 1
        nm = "%s_%s%d" % (self.name, name or "t", self.n)
        t = self.stack.enter_context(self.k.nc.sbuf_tensor(nm, list(shape), dt))
        b = self.k.P.reg(Buf(nm, t))
        self.bufs.append(b)
        return b

    def __enter__(self):
        return self

    def __exit__(self, *a):
        if a[0] is None:
            self.k.P.barrier()
            self.k.P.emit()
            self.k.P.release(self.bufs)
        self.stack.close()
        return False


def rows_ap(t2d, r0, nrows_p, ntile, c0, ncols, rstride=1):
    C = t2d.shape[1]
    return bass.AP(tensor=t2d.tensor, offset=t2d.offset + r0 * C + c0,
                   ap=[[rstride * C, nrows_p], [128 * rstride * C, ntile], [1, ncols]])


def wslice_ap(w2d, r0, kc, c0, ncols):
    C = w2d.shape[1]
    return bass.AP(tensor=w2d.tensor, offset=w2d.offset + r0 * C + c0,
                   ap=[[C, 128], [128 * C, kc], [1, ncols]])


class WStream:
    def __init__(self, k, ph, specs, nslots=4, kcmax=16):
        self.k = k
        self.specs = specs
        self.slots = [ph.sb([128, kcmax, 512], BF16, "ws") for _ in range(nslots)]
        self.issued = 0
        self.nslots = nslots

    def get(self, n):
        P = self.k.P
        lim = min(len(self.specs), n + self.nslots - 1)
        while self.issued < lim:
            i = self.issued
            w2d, r0, kc, c0, ncols = self.specs[i]
            sl = self.slots[i % self.nslots]
            P.dma(sl[:, 0:kc, 0:ncols], wslice_ap(w2d, r0, kc, c0, ncols), writes=[sl], owner=sl)
            self.issued += 1
        return self.slots[n % self.nslots]


def load_bc(k, ph, src_row_ap, ncols, dt=F32, name="bc"):
    b = ph.sb([128, ncols], dt, name)
    k.P.dma(b[:, :], src_row_ap.partition_broadcast(128), writes=[b], owner=b)
    return b


def norm_to_hT(k, xs, sub, A_bc, B_bc, scr, hT, banks, h32=None):
    P = k.P
    junk, ss, rstd, t32, hb = scr["junk"], scr["ss"], scr["rstd"], scr["t32"], scr["hb"]
    xin = xs[:, sub, :]
    P.op("act", lambda e: e.activation(out=junk[:, :], in_=xin, func=AF.Square, accum_out=ss[:, 0:1]),
         reads=[xs], writes=[junk, ss])
    rsqrt_ops(k, ss, rstd, 1.0 / D)
    P.op("dve", lambda e: e.scalar_tensor_tensor(out=t32[:, :], in0=xin, scalar=rstd[:, 1:2], in1=A_bc[:, :],
                                                 op0=ALU.mult, op1=ALU.mult),
         reads=[xs, rstd, A_bc], writes=[t32])
    if h32 is None:
        P.op("pool", lambda e: e.tensor_tensor(out=hb[:, :], in0=t32[:, :], in1=B_bc[:, :], op=ALU.add),
             reads=[t32, B_bc], writes=[hb])
    else:
        P.op("pool", lambda e: e.tensor_tensor(out=h32[:, :], in0=t32[:, :], in1=B_bc[:, :], op=ALU.add),
             reads=[t32, B_bc], writes=[h32])
        P.op("act", lambda e: e.activation(out=hb[:, :], in_=h32[:, :], func=AF.Copy), reads=[h32], writes=[hb])
    transpose_to(k, hb, 0, 16, hT, 0, sub * 128, banks)


def transpose_to(k, src, c0, nchunk, dstT, kc0, t0, banks, eng="dve"):
    P = k.P
    j = 0
    g = 0
    while j < nchunk:
        n = min(8, nchunk - j)
        bank = banks[g % len(banks)]
        bv = k.bview(bank)
        for u in range(n):
            col = c0 + (j + u) * 128
            P.op("pe", lambda e, u=u, col=col, bv=bv: e.transpose(out=bv[:, u * 128:(u + 1) * 128], in_=src[:, col:col + 128],
                                                                  identity=k.identb[:, :]),
                 reads=[src, k.identb], writes=[bank])
        ev = eng if isinstance(eng, str) else eng[g % len(eng)]
        outap = dstT[:, kc0 + j:kc0 + j + n, t0:t0 + 128]
        inap = bv[:, 0:n * 128].rearrange("p (a b) -> p a b", a=n)
        if ev == "act":
            P.op("act", lambda e, outap=outap, inap=inap: e.activation(out=outap, in_=inap, func=AF.Copy),
                 reads=[bank], writes=[dstT])
        else:
            P.op(ev, lambda e, outap=outap, inap=inap: e.tensor_copy(out=outap, in_=inap), reads=[bank], writes=[dstT])
        j += n
        g += 1


def phase_pre(k):
    P = k.P
    with Phase(k, "pre") as ph:
        ccol = ph.sb([128, 16], F32)
        cs = ph.sb([128, 16], F32)
        import os
        if int(os.environ.get("MK_PRE_N", "96")) < 0:
            return
        csb = load_bc(k, ph, k.c[0:1, :], 2048)
        csl = ph.sb([128, 2048], F32)
        tmpd = ph.sb([128, 16, 128], F32)
        P.op("act", lambda e: e.activation(out=csl[:, :], in_=csb[:, :], func=AF.Silu), reads=[csb], writes=[csl])
        P.op("dve", lambda e: e.tensor_tensor(out=tmpd[:, :, :], in0=csl[:, :].rearrange("p (a b) -> p a b", a=16),
                                              in1=k.identf[:, :].unsqueeze(1).broadcast_to([128, 16, 128]), op=ALU.mult),
             reads=[csl, k.identf], writes=[tmpd])
        P.op("dve", lambda e: e.reduce_sum(out=cs[:, :], in_=tmpd[:, :, :], axis=AX.X), reads=[tmpd], writes=[cs])
        wsl = [ph.sb([128, 16, 512], F32, "aw") for _ in range(2)]
        brow = [ph.sb([1, 512], F32, "br") for _ in range(2)]
        nrow = [ph.sb([1, 512], F32, "nr") for _ in range(2)]
        r1 = [ph.sb([1, 512], F32, "r1") for _ in range(2)]
        r2 = [ph.sb([1, 512], F32, "r2") for _ in range(2)]
        n = 0
        import os
        lim = int(os.environ.get("MK_PRE_N", "96"))
        for i in range(4):
            for s in range(24):
                if n >= lim:
                    break
                w = wsl[n % 2]
                bank = k.banks[n % 2]
                kind, sub = s // 4, s % 4
                P.dma(w[:, :, :], wslice_ap(k.ada_w, i * 2048, 16, s * 512, 512), writes=[w], owner=w)
                b = brow[n % 2]
                P.dma(b[:, :], k.ada_b[i:i + 1, s * 512:(s + 1) * 512], writes=[b], owner=b)
                for kc in range(16):
                    P.op("pe", lambda e, kc=kc, w=w, bank=bank: e.matmul(bank[0:1, 0:512], cs[:, kc:kc + 1], w[:, kc, :],
                                                                        start=(kc == 0), stop=(kc == 15)),
                         reads=[cs, w], writes=[bank])
                o2 = r2[n % 2]
                if kind in (1, 4):
                    nr = nrow[n % 2]
                    src = (k.norm_mix if kind == 1 else k.norm_ffn)[i:i + 1, sub * 512:(sub + 1) * 512]
                    P.dma(nr[:, :], src, writes=[nr], owner=nr)
                    o1 = r1[n % 2]
                    P.op("dve", lambda e, o1=o1, bank=bank, b=b: e.tensor_tensor(out=o1[:, :], in0=bank[0:1, 0:512], in1=b[:, :], op=ALU.add),
                         reads=[bank, b], writes=[o1])
                    P.op("dve", lambda e, o1=o1, o2=o2, nr=nr: e.scalar_tensor_tensor(out=o2[:, :], in0=o1[:, :], scalar=1.0, in1=nr[:, :],
                                                                                     op0=ALU.add, op1=ALU.mult),
                         reads=[o1, nr], writes=[o2])
                else:
                    P.op("dve", lambda e, o2=o2, bank=bank, b=b: e.tensor_tensor(out=o2[:, :], in0=bank[0:1, 0:512], in1=b[:, :], op=ALU.add),
                         reads=[bank, b], writes=[o2])
                P.dma(k.MODS[i * 6 + kind:i * 6 + kind + 1, sub * 512:(sub + 1) * 512], o2[:, :], reads=[o2], owner=o2, load=False)
                n += 1


def phase_wconv(k, jobs):
    P = k.P
    with Phase(k, "wc") as ph:
        win = [ph.sb([128, 4, 2048], F32, "wi") for _ in range(2)]
        wo = [ph.sb([128, 4, 2048], BF16, "wo") for _ in range(2)]
        gbc = [ph.sb([128, 2048], F32, "g") for _ in range(2)]
        n = 0
        ng = 0
        for (src, dst, grow) in jobs:
            R, C = src.shape
            g = None
            if grow is not None:
                g = gbc[ng % 2]
                ng += 1
                P.dma(g[:, :], k.MODS[grow:grow + 1, :].partition_broadcast(128), writes=[g], owner=g)
            for r0 in range(0, R, 512):
                for c0 in range(0, C, 2048):
                    cw = min(2048, C - c0)
                    a = win[n % 2]
                    o = wo[n % 2]
                    P.dma(a[:, :, 0:cw], wslice_ap(src, r0, 4, c0, cw), writes=[a], owner=a)
                    if g is not None:
                        eng = ("dve", "pool")[n % 2]
                        gin = g[:, c0:c0 + cw].unsqueeze(1).broadcast_to([128, 4, cw])
                        P.op(eng, lambda e, a=a, o=o, gin=gin, cw=cw: e.tensor_tensor(out=o[:, :, 0:cw], in0=a[:, :, 0:cw], in1=gin, op=ALU.mult),
                             reads=[a, g], writes=[o])
                    else:
                        eng = ("dve", "act", "pool")[n % 3]
                        if eng == "act":
                            P.op("act", lambda e, a=a, o=o, cw=cw: e.activation(out=o[:, :, 0:cw], in_=a[:, :, 0:cw], func=AF.Copy),
                                 reads=[a], writes=[o])
                        else:
                            P.op(eng, lambda e, a=a, o=o, cw=cw: e.tensor_copy(out=o[:, :, 0:cw], in_=a[:, :, 0:cw]), reads=[a], writes=[o])
                    P.dma(wslice_ap(dst, r0, 4, c0, cw), o[:, :, 0:cw], reads=[o], owner=o, load=False)
                    n += 1


def norm_scratch(ph):
    hb = ph.sb([128, 2048], BF16, "hb")
    return {"junk": hb, "ss": ph.sb([128, 1], F32, "ss"), "rstd": ph.sb([128, 2], F32, "rstd"),
            "t32": ph.sb([128, 2048], F32, "t32"), "hb": hb}


def phase_qkv(k, layer, win_bf, W, NR, qkv):
    P = k.P
    S = k.S
    with Phase(k, "p%d" % layer) as ph:
        A_bc = load_bc(k, ph, k.MODS[layer * 6 + 1:layer * 6 + 2, :], 2048)
        B_bc = load_bc(k, ph, k.MODS[layer * 6 + 0:layer * 6 + 1, :], 2048)
        xs2 = [ph.sb([128, 4, 2048], F32, "xs") for _ in range(2)]
        rp2 = [ph.sb([128, 4, 32], F32, "rp") for _ in range(2)]
        hT2 = [ph.sb([128, 16, 512], BF16, "hT") for _ in range(2)]
        scr = norm_scratch(ph)
        stage = [ph.sb([128, 4, 512], BF16, "st") for _ in range(2)]
        tcos = [ph.sb([128, 4, 2, 16], F32, "tc") for _ in range(2)]
        tsin = [ph.sb([128, 4, 2, 16], F32, "ts") for _ in range(2)]
        nfs = W // 512
        ntt = S // 512
        specs = []
        for tt in range(ntt):
            for fs in range(nfs):
                specs.append((win_bf, 0, 16, fs * 512, 512))
        ws = WStream(k, ph, specs, nslots=4)
        tbanks = [k.banks[6], k.banks[7]]
        n = 0
        nst = 0
        for tt in range(ntt):
            xs = xs2[tt % 2]
            rp = rp2[tt % 2]
            hT = hT2[tt % 2]
            P.dma(xs[:, :, :], rows_ap(k.XR if layer > 0 else k.x, tt * 512, 128, 4, 0, 2048), writes=[xs], owner=xs)
            P.dma(rp[:, :, :], rows_ap(k.rope, tt * 512, 128, 4, 0, 32), writes=[rp], owner=rp)
            for sub in range(4):
                norm_to_hT(k, xs, sub, A_bc, B_bc, scr, hT, tbanks)
            for fs in range(nfs):
                w = ws.get(n)
                n += 1
                st = stage[nst % 2]
                nst += 1
                for sub in range(4):
                    bank = k.banks[(fs * 4 + sub) % 6]
                    for kc in range(16):
                        P.op("pe", lambda e, kc=kc, w=w, bank=bank, hT=hT, sub=sub: e.matmul(
                            bank[:, 0:512], hT[:, kc, sub * 128:(sub + 1) * 128], w[:, kc, :], start=(kc == 0), stop=(kc == 15)),
                            reads=[hT, w], writes=[bank])
                    P.op("act", lambda e, st=st, sub=sub, bank=bank: e.activation(out=st[:, sub, :], in_=bank[:, 0:512], func=AF.Copy),
                         reads=[bank], writes=[st])
                    if fs * 512 < NR:
                        tc = tcos[sub % 2]
                        tsn = tsin[sub % 2]
                        x12 = bank[:, 0:512].rearrange("p (h d) -> p h d", h=4)[:, :, 0:32].rearrange("p h (two j) -> p h two j", two=2)
                        cosb = rp[:, sub, 0:16].unsqueeze(1).unsqueeze(1).broadcast_to([128, 4, 2, 16])
                        sinb = rp[:, sub, 16:32].unsqueeze(1).unsqueeze(1).broadcast_to([128, 4, 2, 16])
                        P.op("dve", lambda e, tc=tc, x12=x12, cosb=cosb: e.tensor_tensor(out=tc[:, :, :, :], in0=x12, in1=cosb, op=ALU.mult),
                             reads=[bank, rp], writes=[tc])
                        P.op("dve", lambda e, tsn=tsn, x12=x12, sinb=sinb: e.tensor_tensor(out=tsn[:, :, :, :], in0=x12, in1=sinb, op=ALU.mult),
                             reads=[bank, rp], writes=[tsn])
                        sv = st[:, sub, :].rearrange("p (h d) -> p h d", h=4)
                        P.op("pool", lambda e, sv=sv, tc=tc, tsn=tsn: e.tensor_tensor(out=sv[:, :, 0:16], in0=tc[:, :, 0, :], in1=tsn[:, :, 1, :], op=ALU.subtract),
                             reads=[tc, tsn], writes=[st])
                        P.op("pool", lambda e, sv=sv, tc=tc, tsn=tsn: e.tensor_tensor(out=sv[:, :, 16:32], in0=tc[:, :, 1, :], in1=tsn[:, :, 0, :], op=ALU.add),
                             reads=[tc, tsn], writes=[st])
                P.dma(rows_ap(qkv, tt * 512, 128, 4, fs * 512, 512), st[:, :, :], reads=[st], owner=st, load=False)


def rsqrt_ops(k, ss, rstd, scale):
    P = k.P
    P.op("act", lambda e: e.activation(out=rstd[:, 0:1], in_=ss[:, 0:1], func=AF.Ln, scale=scale, bias=EPS), reads=[ss], writes=[rstd])
    P.op("act", lambda e: e.activation(out=rstd[:, 1:2], in_=rstd[:, 0:1], func=AF.Exp, scale=-0.5), reads=[rstd], writes=[rstd])


def phase_att_a(k, layer, j, qkv, lam0):
    P = k.P
    S = k.S
    NT = S // 128
    NQT = S // 512
    sc = 128.0 ** -0.5
    with Phase(k, "aa%d" % layer) as ph:
        lamb = load_bc(k, ph, k.a_lambda[j:j + 1, :], 512)
        prod = ph.sb([128, 2, 128], F32)
        sums = ph.sb([128, 2], F32)
        es = ph.sb([128, 2], F32)
        nlam = ph.sb([128, 1], F32)
        P.op("dve", lambda e: e.tensor_tensor(out=prod[:, 0, :], in0=lamb[:, 0:128], in1=lamb[:, 128:256], op=ALU.mult), reads=[lamb], writes=[prod])
        P.op("dve", lambda e: e.tensor_tensor(out=prod[:, 1, :], in0=lamb[:, 256:384], in1=lamb[:, 384:512], op=ALU.mult), reads=[lamb], writes=[prod])
        P.op("dve", lambda e: e.reduce_sum(out=sums[:, 0:2], in_=prod[:, :, :], axis=AX.X), reads=[prod], writes=[sums])
        P.op("act", lambda e: e.activation(out=es[:, :], in_=sums[:, :], func=AF.Exp), reads=[sums], writes=[es])
        P.op("dve", lambda e: e.tensor_tensor(out=nlam[:, :], in0=es[:, 1:2], in1=es[:, 0:1], op=ALU.subtract), reads=[es], writes=[nlam])
        P.op("dve", lambda e: e.tensor_scalar_add(out=nlam[:, :], in0=nlam[:, :], scalar1=-lam0), reads=[nlam], writes=[nlam])
        subln = load_bc(k, ph, k.a_subln[j:j + 1, :], 256)
        P.op("dve", lambda e: e.tensor_scalar_mul(out=subln[:, :], in0=subln[:, :], scalar1=1.0 - lam0), reads=[subln], writes=[subln])

        kst = [ph.sb([128, NT, 256], BF16, "kst") for _ in range(2)]
        vaug = [ph.sb([128, NT, 272], BF16, "va") for _ in range(2)]
        for v in vaug:
            P.op("pool", lambda e, v=v: e.memset(v[:, :, 256:260], 1.0), writes=[v])
        KT = ph.sb([128, 2, S], BF16, "KT")
        qst = [ph.sb([128, 4, 256], BF16, "qst") for _ in range(2)]
        QT = [ph.sb([128, 2, 512], BF16, "QT") for _ in range(2)]
        PT = [ph.sb([128, 512], BF16, "PT") for _ in range(3)]
        ub = [ph.sb([128, 256], F32, "ub") for _ in range(4)]
        u2 = [ph.sb([128, 256], F32, "u2") for _ in range(2)]
        junk = ph.sb([128, 256], F32, "junk")
        rr = [ph.sb([128, 2], F32, "rr") for _ in range(2)]
        ssq = [ph.sb([128, 1], F32, "ssq") for _ in range(2)]
        rstd = [ph.sb([128, 2], F32, "rstd") for _ in range(2)]
        ost = [ph.sb([128, 4, 256], BF16, "ost") for _ in range(2)]
        acc = k.banks[0:4]
        sT = k.banks[4:6]
        tb = k.banks[6:8]

        def load_head(h):
            for t0 in range(0, NT, 16):
                tn = min(16, NT - t0)
                P.dma(kst[h % 2][:, t0:t0 + tn, :], rows_ap(qkv, t0 * 128, 128, tn, 2048 + h * 256, 256), writes=[kst[h % 2]], owner=kst[h % 2])
                P.dma(vaug[h % 2][:, t0:t0 + tn, 0:256], rows_ap(qkv, t0 * 128, 128, tn, 4096 + h * 256, 256), writes=[vaug[h % 2]], owner=vaug[h % 2])

        def load_q(h, qt, n):
            P.dma(qst[n % 2][:, :, :], rows_ap(qkv, qt * 512, 128, 4, h * 256, 256), writes=[qst[n % 2]], owner=qst[n % 2])

        load_head(0)
        nq = 0
        fin = 0
        for h in range(8):
            ks = kst[h % 2]
            va = vaug[h % 2]
            load_q(h, 0, nq)
            g = 0
            for m in range(2):
                for tg in range((NT + 7) // 8):
                    bank = tb[g % 2]
                    bv = k.bview(bank)
                    nn = min(8, NT - tg * 8)
                    for u in range(nn):
                        t = tg * 8 + u
                        P.op("pe", lambda e, bv=bv, u=u, t=t, m=m, ks=ks: e.transpose(out=bv[:, u * 128:(u + 1) * 128], in_=ks[:, t, m * 128:(m + 1) * 128],
                                                                                      identity=k.identb[:, :]),
                             reads=[ks, k.identb], writes=[bank])
                    P.op("dve", lambda e, bv=bv, m=m, tg=tg: e.tensor_copy(out=KT[:, m, tg * 1024:tg * 1024 + nn * 128], in_=bv[:, 0:nn * 128]),
                         reads=[bank], writes=[KT])
                    g += 1
            if h + 1 < 8:
                load_head(h + 1)
            for qt in range(NQT):
                qs = qst[nq % 2]
                qT = QT[nq % 2]
                nq += 1
                if qt + 1 < NQT:
                    load_q(h, qt + 1, nq)
                bank = tb[g % 2]
                g += 1
                bv = k.bview(bank)
                for m in range(2):
                    for s in range(4):
                        u = m * 4 + s
                        P.op("pe", lambda e, bv=bv, u=u, s=s, m=m, qs=qs: e.transpose(out=bv[:, u * 128:(u + 1) * 128], in_=qs[:, s, m * 128:(m + 1) * 128],
                                                                                      identity=k.identb[:, :]),
                             reads=[qs, k.identb], writes=[bank])
                P.op("dve", lambda e, bv=bv, qT=qT: e.tensor_copy(out=qT[:, :, :], in_=bv[:, 0:1024].rearrange("p (m q) -> p m q", m=2)),
                     reads=[bank], writes=[qT])
                os_ = ost[qt % 2]
                for m in range(2):
                    def QK(kt, m=m, qT=qT):
                        b = sT[kt % 2]
                        P.op("pe", lambda e, b=b, kt=kt: e.matmul(b[:, 0:512], KT[:, m, kt * 128:(kt + 1) * 128], qT[:, m, :], start=True, stop=True),
                             reads=[KT, qT], writes=[b])
                        pt = PT[kt % 3]
                        P.op("act", lambda e, b=b, pt=pt: e.activation(out=pt[:, :], in_=b[:, 0:512], func=AF.Exp, scale=sc), reads=[b], writes=[pt])

                    def PV(kt, va=va):
                        pt = PT[kt % 3]
                        for s in range(4):
                            P.op("pe", lambda e, s=s, pt=pt, kt=kt: e.matmul(acc[s][:, 0:260], pt[:, s * 128:(s + 1) * 128], va[:, kt, 0:260],
                                                                            start=(kt == 0), stop=(kt == NT - 1)),
                                 reads=[pt, va], writes=[acc[s]])

                    QK(0)
                    if NT > 1:
                        QK(1)
                    for kt in range(NT):
                        PV(kt)
                        if kt + 2 < NT:
                            QK(kt + 2)
                    for s in range(4):
                        a = acc[s]
                        r = rr[fin % 2]
                        if m == 0:
                            P.op("dve", lambda e, a=a, r=r: e.reciprocal(out=r[:, 0:1], in_=a[:, 256:257]), reads=[a], writes=[r])
                            P.op("dve", lambda e, a=a, r=r, s=s: e.tensor_scalar_mul(out=ub[s][:, :], in0=a[:, 0:256], scalar1=r[:, 0:1]),
                                 reads=[a, r], writes=[ub[s]])
                        else:
                            uu = u2[fin % 2]
                            sq = ssq[fin % 2]
                            rs = rstd[fin % 2]
                            P.op("dve", lambda e, a=a, r=r: e.reciprocal(out=r[:, 0:1], in_=a[:, 256:257]), reads=[a], writes=[r])
                            P.op("dve", lambda e, r=r: e.tensor_tensor(out=r[:, 1:2], in0=r[:, 0:1], in1=nlam[:, 0:1], op=ALU.mult), reads=[r, nlam], writes=[r])
                            P.op("dve", lambda e, a=a, r=r, s=s, uu=uu: e.scalar_tensor_tensor(out=uu[:, :], in0=a[:, 0:256], scalar=r[:, 1:2], in1=ub[s][:, :],
                                                                                             op0=ALU.mult, op1=ALU.add),
                                 reads=[a, r, ub[s]], writes=[uu])
                            P.op("act", lambda e, uu=uu, sq=sq: e.activation(out=junk[:, :], in_=uu[:, :], func=AF.Square, accum_out=sq[:, 0:1]),
                                 reads=[uu], writes=[junk, sq])
                            rsqrt_ops(k, sq, rs, 1.0 / 256)
                            P.op("dve", lambda e, uu=uu, rs=rs, s=s, os_=os_: e.scalar_tensor_tensor(out=os_[:, s, :], in0=uu[:, :], scalar=rs[:, 1:2], in1=subln[:, :],
                                                                                                   op0=ALU.mult, op1=ALU.mult),
                                 reads=[uu, rs, subln], writes=[os_])
                        fin += 1
                P.dma(rows_ap(k.O, qt * 512, 128, 4, h * 256, 256), os_[:, :, :], reads=[os_], owner=os_, load=False)
        if k.DBG is not None:
            dsb = ph.sb([128, 8], F32, "dsb")
            P.op("dve", lambda e: e.tensor_copy(out=dsb[:, 0:4], in_=acc[0][:, 256:260]), reads=[acc[0]], writes=[dsb])
            P.op("dve", lambda e: e.tensor_copy(out=dsb[:, 4:8], in_=acc[0][:, 0:4]), reads=[acc[0]], writes=[dsb])
            for (b_, c0, w_) in ((nlam, 0, 1), (es, 1, 2), (sums, 3, 2), (dsb, 16, 8), (subln, 32, 256), (rr[0], 288, 2), (rstd[0], 290, 2), (ssq[0], 292, 1),
                                 (ub[0], 300, 256), (u2[0], 556, 256)):
                P.dma(k.DBG[:, c0:c0 + w_], b_[:, 0:w_], reads=[b_], owner=b_, load=False, allow_slow_non_contiguous=True)


def phase_att_b(k, qkv):
    P = k.P
    S = k.S
    NT = S // 128
    sc = 128.0 ** -0.5
    with Phase(k, "ab") as ph:
        es16 = load_bc(k, ph, k.b_sink[0:1, :], 16)
        P.op("act", lambda e: e.activation(out=es16[:, :], in_=es16[:, :], func=AF.Exp), reads=[es16], writes=[es16])
        kst = [ph.sb([128, NT, 128], BF16, "kst") for _ in range(2)]
        vaug = [ph.sb([128, NT, 144], BF16, "va") for _ in range(2)]
        for v in vaug:
            P.op("pool", lambda e, v=v: e.memset(v[:, :, 128:132], 1.0), writes=[v])
        KT = ph.sb([128, S], BF16, "KT")
        qst = [ph.sb([128, 512], BF16, "qst") for _ in range(2)]
        QT = [ph.sb([128, 512], BF16, "QT") for _ in range(2)]
        PT = [[ph.sb([128, 512], BF16, "PT") for _ in range(3)] for _ in range(2)]
        dt = [ph.sb([128, 4], F32, "dt") for _ in range(2)]
        ost = [ph.sb([128, 4, 128], BF16, "ost") for _ in range(2)]
        accs = [k.banks[0:2], k.banks[2:4]]
        sT = k.banks[4:7]
        tbank = k.banks[7]

        def load_head(g):
            for t0 in range(0, NT, 16):
                tn = min(16, NT - t0)
                P.dma(kst[g % 2][:, t0:t0 + tn, :], rows_ap(qkv, t0 * 128, 128, tn, 2048 + g * 128, 128), writes=[kst[g % 2]], owner=kst[g % 2])
                P.dma(vaug[g % 2][:, t0:t0 + tn, 0:128], rows_ap(qkv, t0 * 128, 128, tn, 2560 + g * 128, 128), writes=[vaug[g % 2]], owner=vaug[g % 2])

        def load_q(g, qb, n):
            P.dma(qst[n % 2][:, :], rows_ap(qkv, qb * 128, 128, 1, g * 512, 512)[:, 0, :], writes=[qst[n % 2]], owner=qst[n % 2])

        load_head(0)
        nq = 0
        for g in range(4):
            ks = kst[g % 2]
            va = vaug[g % 2]
            load_q(g, 0, nq)
            bv = k.bview(tbank)
            for tg in range((NT + 7) // 8):
                nn = min(8, NT - tg * 8)
                for u in range(nn):
                    t = tg * 8 + u
                    P.op("pe", lambda e, u=u, t=t, ks=ks: e.transpose(out=bv[:, u * 128:(u + 1) * 128], in_=ks[:, t, :], identity=k.identb[:, :]),
                         reads=[ks, k.identb], writes=[tbank])
                P.op("dve", lambda e, tg=tg: e.tensor_copy(out=KT[:, tg * 1024:tg * 1024 + nn * 128], in_=bv[:, 0:nn * 128]), reads=[tbank], writes=[KT])
            if g + 1 < 4:
                load_head(g + 1)

            def front(qb, n):
                qs = qst[n % 2]
                qT = QT[n % 2]
                for u in range(4):
                    P.op("pe", lambda e, u=u, qs=qs: e.transpose(out=bv[:, u * 128:(u + 1) * 128], in_=qs[:, u * 128:(u + 1) * 128], identity=k.identb[:, :]),
                         reads=[qs, k.identb], writes=[tbank])
                P.op("dve", lambda e, qT=qT: e.tensor_copy(out=qT[:, :], in_=bv[:, 0:512]), reads=[tbank], writes=[qT])
                kbs = [kb for kb in (qb - 1, qb, qb + 1) if 0 <= kb < NT]
                for idx, kb in enumerate(kbs):
                    b = sT[idx]
                    pt = PT[n % 2][idx]
                    P.op("pe", lambda e, b=b, kb=kb, qT=qT: e.matmul(b[:, 0:512], KT[:, kb * 128:(kb + 1) * 128], qT[:, :], start=True, stop=True),
                         reads=[KT, qT], writes=[b])
                    P.op("act", lambda e, b=b, pt=pt: e.activation(out=pt[:, :], in_=b[:, 0:512], func=AF.Exp, scale=sc), reads=[b], writes=[pt])
                    if kb != qb:
                        mi = 0 if kb == qb - 1 else 1
                        mk = k.maskAB[:, mi, :].unsqueeze(1).broadcast_to([128, 4, 128])
                        pv = pt[:, :].rearrange("p (h q) -> p h q", h=4)
                        P.op("pool", lambda e, pv=pv, mk=mk: e.tensor_tensor(out=pv, in0=pv, in1=mk, op=ALU.mult), reads=[pt, k.maskAB], writes=[pt])
                return kbs

            def back(qb, n, kbs):
                ac = accs[n % 2]
                for hq in range(4):
                    bank = ac[hq // 2]
                    c0 = (hq % 2) * 132
                    for idx, kb in enumerate(kbs):
                        pt = PT[n % 2][idx]
                        P.op("pe", lambda e, bank=bank, c0=c0, pt=pt, hq=hq, kb=kb, idx=idx: e.matmul(
                            bank[:, c0:c0 + 132], pt[:, hq * 128:(hq + 1) * 128], va[:, kb, 0:132], start=(idx == 0), stop=(idx == len(kbs) - 1)),
                            reads=[pt, va], writes=[bank])
                d = dt[n % 2]
                o = ost[n % 2]
                for bi in range(2):
                    bank = ac[bi]
                    v3 = bank[:, 0:264].rearrange("p (h c) -> p h c", h=2)
                    P.op("dve", lambda e, d=d, v3=v3, bi=bi: e.tensor_tensor(out=d[:, 2 * bi:2 * bi + 2].unsqueeze(2), in0=v3[:, :, 128:129],
                                                                             in1=es16[:, g * 4 + 2 * bi:g * 4 + 2 * bi + 2].unsqueeze(2), op=ALU.add),
                         reads=[bank, es16], writes=[d])
                    P.op("dve", lambda e, d=d, bi=bi: e.reciprocal(out=d[:, 2 * bi:2 * bi + 2], in_=d[:, 2 * bi:2 * bi + 2]), reads=[d], writes=[d])
                    P.op("dve", lambda e, d=d, v3=v3, bi=bi, o=o: e.tensor_tensor(out=o[:, 2 * bi:2 * bi + 2, :], in0=v3[:, :, 0:128],
                                                                                 in1=d[:, 2 * bi:2 * bi + 2].unsqueeze(2).broadcast_to([128, 2, 128]), op=ALU.mult),
                         reads=[bank, d], writes=[o])
                P.dma(rows_ap(k.O, qb * 128, 128, 1, g * 512, 512)[:, 0, :], o[:, :, :].rearrange("p h d -> p (h d)"), reads=[o], owner=o, load=False)

            prev = None
            for qb in range(NT):
                n = nq
                nq += 1
                if qb + 1 < NT:
                    load_q(g, qb + 1, nq)
                kbs = front(qb, n)
                if prev is not None:
                    back(*prev)
                prev = (qb, n, kbs)
            back(*prev)


C_DILS = (1, 4, 16)


def phase_att_c(k, qkv):
    P = k.P
    S = k.S
    sc = 128.0 ** -0.5
    NTM = S // 128
    W = 9216
    with Phase(k, "ac") as ph:
        kst = [ph.sb([128, NTM + 1, 128], BF16, "kst") for _ in range(2)]
        vst = [ph.sb([128, NTM + 1, 144], BF16, "vst") for _ in range(2)]
        qst = [ph.sb([128, NTM, 128], BF16, "qst") for _ in range(2)]
        KT = ph.sb([128, (NTM + 1) * 128], BF16, "KT")
        QT = ph.sb([128, NTM * 128], BF16, "QT")
        PT = [ph.sb([128, 256], BF16, "PT") for _ in range(3)]
        nst = [ph.sb([128, NTM, 129], F32, "nst") for _ in range(1)]
        for v in vst:
            P.op("pool", lambda e, v=v: e.memset(v[:, :, 128:132], 1.0), writes=[v])
        sT = k.banks[0:2]
        acc = k.banks[2:4]
        tb = k.banks[6:8]
        n = 0
        g = 0
        for gi, dil in enumerate(C_DILS):
            L = S // dil
            nt = L // 128
            for b in range(2):
                for t_ in (kst[b], vst[b]):
                    P.op("pool", lambda e, t_=t_: e.memset(t_[0:64, 0, 0:128], 0.0), writes=[t_])
                    P.op("pool", lambda e, t_=t_, nt=nt: e.memset(t_[64:128, nt, 0:128], 0.0), writes=[t_])
            for r in range(dil):
                for hd in range(8):
                    cq = gi * 1024 + hd * 128
                    ks, vs, qs = kst[n % 2], vst[n % 2], qst[n % 2]
                    ns = nst[0]
                    n += 1
                    for (dstb, c0) in ((ks, 3072 + cq), (vs, 6144 + cq)):
                        def src(l0, npart, ntile, c0=c0):
                            return bass.AP(tensor=qkv.tensor, offset=qkv.offset + (r + dil * l0) * W + c0,
                                           ap=[[dil * W, npart], [128 * dil * W, ntile], [1, 128]])
                        P.dma(dstb[64:128, 0:1, 0:128], src(0, 64, 1), writes=[dstb], owner=dstb)
                        for t0 in range(1, nt, 16):
                            tn = min(16, nt - t0)
                            P.dma(dstb[:, t0:t0 + tn, 0:128], src(64 + 128 * (t0 - 1), 128, tn), writes=[dstb], owner=dstb)
                        P.dma(dstb[0:64, nt:nt + 1, 0:128], src(L - 64, 64, 1), writes=[dstb], owner=dstb)
                    for t0 in range(0, nt, 16):
                        tn = min(16, nt - t0)
                        qsrc = bass.AP(tensor=qkv.tensor, offset=qkv.offset + (r + dil * 128 * t0) * W + cq, ap=[[dil * W, 128], [128 * dil * W, tn], [1, 128]])
                        P.dma(qs[:, t0:t0 + tn, :], qsrc, writes=[qs], owner=qs)
                    for (srcb, dstT, cnt) in ((ks, KT, nt + 1), (qs, QT, nt)):
                        j = 0
                        while j < cnt:
                            m = min(8, cnt - j)
                            bank = tb[g % 2]
                            g += 1
                            bv = k.bview(bank)
                            for u in range(m):
                                P.op("pe", lambda e, bv=bv, u=u, srcb=srcb, j=j: e.transpose(out=bv[:, u * 128:(u + 1) * 128], in_=srcb[:, j + u, 0:128],
                                                                                           identity=k.identb[:, :]),
                                     reads=[srcb, k.identb], writes=[bank])
                            P.op("dve", lambda e, bv=bv, dstT=dstT, j=j, m=m: e.tensor_copy(out=dstT[:, j * 128:(j + m) * 128], in_=bv[:, 0:m * 128]),
                                 reads=[bank], writes=[dstT])
                            j += m

                    def front(j):
                        b = sT[j % 2]
                        pt = PT[j % 3]
                        P.op("pe", lambda e: e.matmul(b[:, 0:128], KT[:, j * 128:(j + 1) * 128], QT[:, j * 128:(j + 1) * 128], start=True, stop=True),
                             reads=[KT, QT], writes=[b])
                        P.op("pe", lambda e: e.matmul(b[:, 128:256], KT[:, (j + 1) * 128:(j + 2) * 128], QT[:, j * 128:(j + 1) * 128], start=True, stop=True),
                             reads=[KT, QT], writes=[b])
                        P.op("act", lambda e: e.activation(out=pt[:, :], in_=b[:, 0:256], func=AF.Exp, scale=sc), reads=[b], writes=[pt])
                        v = (1 if j == 0 else 0) + (2 if j == nt - 1 else 0)
                        P.op("pool", lambda e: e.tensor_tensor(out=pt[:, :], in0=pt[:, :], in1=k.maskC[:, v, :], op=ALU.mult), reads=[pt, k.maskC], writes=[pt])

                    def back(j):
                        pt = PT[j % 3]
                        a = acc[j % 2]
                        P.op("pe", lambda e: e.matmul(a[:, 0:132], pt[:, 0:128], vs[:, j, 0:132], start=True, stop=False), reads=[pt, vs], writes=[a])
                        P.op("pe", lambda e: e.matmul(a[:, 0:132], pt[:, 128:256], vs[:, j + 1, 0:132], start=False, stop=True), reads=[pt, vs], writes=[a])
                        P.op("dve", lambda e: e.tensor_copy(out=ns[:, j, :], in_=a[:, 0:129]), reads=[a], writes=[ns])

                    front(0)
                    for j in range(nt):
                        if j + 1 < nt:
                            front(j + 1)
                        back(j)
                    for t0 in range(0, nt, 16):
                        tn = min(16, nt - t0)
                        dst = bass.AP(tensor=k.NUMD[gi].tensor, offset=k.NUMD[gi].offset + (r + dil * 128 * t0) * 1032 + hd * 129,
                                      ap=[[dil * 1032, 128], [128 * dil * 1032, tn], [1, 129]])
                        P.dma(dst, ns[:, t0:t0 + tn, :], reads=[ns], owner=ns, load=False)


def phase_post(k, layer, kind, OW, wout_bf, ffn, last):
    P = k.P
    S = k.S
    ntt = S // 512
    KCO = OW // 128
    moe = ffn[0] == "moe"
    with Phase(k, "po%d" % layer) as ph:
        A_bc = load_bc(k, ph, k.MODS[layer * 6 + 4:layer * 6 + 5, :], 2048)
        B_bc = load_bc(k, ph, k.MODS[layer * 6 + 3:layer * 6 + 4, :], 2048)
        fn_bc = load_bc(k, ph, k.final_norm[0:1, :], 2048) if last else None
        xs = ph.sb([128, 4, 2048], F32, "xs")
        ost = [ph.sb([128, OW], BF16, "ost") for _ in range(1)]
        hT = ph.sb([128, 16, 512], BF16, "hT")
        actT = ph.sb([128, 8 if moe else 32, 512], BF16, "actT")
        scr = norm_scratch(ph)
        sg2 = [ph.sb([128, 512], BF16, "sg") for _ in range(2)]
        if kind == "c":
            nld = [[ph.sb([128, 8, 129], F32, "nl") for _ in range(3)] for _ in range(1)]
            rden = ph.sb([128, 8], F32, "rden")
        if moe:
            hT32 = ph.sb([128, 16, 128], F32, "hT32")
            lg = ph.sb([128, 8], F32, "lg")
            t8 = [ph.sb([128, 8], F32, "t8") for _ in range(4)]
            sm = ph.sb([128, 4], F32, "sm")
            gates = ph.sb([128, 4, 8], F32, "gates")
            rt = ph.sb([128, 16, 8], F32, "rt")
            P.dma(rt[:, :, :], wslice_ap(k.moe_router, ffn[1] * 2048, 16, 0, 8), writes=[rt], owner=rt)
        if last:
            yst = [ph.sb([128, 2048], F32, "yst") for _ in range(2)]
            ss2 = [ph.sb([128, 1], F32, "ss2") for _ in range(2)]
            rs2 = [ph.sb([128, 2], F32, "rs2") for _ in range(2)]
        specs = []
        for tt in range(ntt):
            for fs in range(4):
                specs.append((wout_bf, 0, KCO, fs * 512, 512))
            if not moe:
                _, wgu, wdn = ffn
                for jg in range(8):
                    specs.append((wgu, 0, 16, jg * 512, 512))
                    specs.append((wgu, 0, 16, 4096 + jg * 512, 512))
                for fs in range(4):
                    for half in range(2):
                        specs.append((wdn, half * 2048, 16, fs * 512, 512))
            else:
                _, mi, wgu, wdn = ffn
                for ex in range(8):
                    for pr in range(2):
                        specs.append((wgu, (mi * 8 + ex) * 2048, 16, pr * 512, 512))
                        specs.append((wgu, (mi * 8 + ex) * 2048, 16, 1024 + pr * 512, 512))
                    for fs in range(4):
                        specs.append((wdn, (mi * 8 + ex) * 1024, 8, fs * 512, 512))
        ws = WStream(k, ph, specs, nslots=4)
        tbanks = k.banks[6:8]
        n = 0
        cnt = 0
        nsg = 0
        for tt in range(ntt):
            P.dma(xs[:, :, :], rows_ap(k.XR if layer > 0 else k.x, tt * 512, 128, 4, 0, 2048), writes=[xs], owner=xs)
            for sub in range(4):
                o = ost[0]
                row0 = tt * 512 + sub * 128
                if kind != "c":
                    P.dma(o[:, :], rows_ap(k.O, row0, 128, 1, 0, OW)[:, 0, :], writes=[o], owner=o)
                else:
                    nl = nld[0]
                    for gi in range(3):
                        P.dma(nl[gi][:, :, :], rows_ap(k.NUMD[gi], row0, 128, 1, 0, 1032)[:, 0, :].rearrange("p (h c) -> p h c", h=8),
                              writes=[nl[gi]], owner=nl[gi])
                    P.op("dve", lambda e: e.tensor_tensor(out=nl[0][:, :, :], in0=nl[0][:, :, :], in1=nl[1][:, :, :], op=ALU.add), reads=[nl[0], nl[1]], writes=[nl[0]])
                    P.op("dve", lambda e: e.tensor_tensor(out=nl[0][:, :, :], in0=nl[0][:, :, :], in1=nl[2][:, :, :], op=ALU.add), reads=[nl[0], nl[2]], writes=[nl[0]])
                    P.op("dve", lambda e: e.reciprocal(out=rden[:, :].unsqueeze(2), in_=nl[0][:, :, 128:129]), reads=[nl[0]], writes=[rden])
                    P.op("dve", lambda e: e.tensor_tensor(out=o[:, :].rearrange("p (h d) -> p h d", h=8), in0=nl[0][:, :, 0:128],
                                                          in1=rden[:, :].unsqueeze(2).broadcast_to([128, 8, 128]), op=ALU.mult),
                         reads=[nl[0], rden], writes=[o])
                transpose_to(k, o, 0, KCO, hT, 0, sub * 128, tbanks)
            for fs in range(4):
                w = ws.get(n)
                n += 1
                for sub in range(4):
                    bank = k.banks[cnt % 6]
                    cnt += 1
                    for kc in range(KCO):
                        P.op("pe", lambda e: e.matmul(bank[:, 0:512], hT[:, kc, sub * 128:(sub + 1) * 128], w[:, kc, :], start=(kc == 0), stop=(kc == KCO - 1)),
                             reads=[hT, w], writes=[bank])
                    xv = xs[:, sub, fs * 512:(fs + 1) * 512]
                    P.op("dve", lambda e: e.tensor_tensor(out=xv, in0=bank[:, 0:512], in1=xv, op=ALU.add), reads=[bank, xs], writes=[xs])
            for sub in range(4):
                norm_to_hT(k, xs, sub, A_bc, B_bc, scr, hT, tbanks, h32=(scr["t32"] if moe else None))
                if moe:
                    t32 = scr["t32"]
                    for q4 in range(4):
                        bank = tbanks[q4 % 2]
                        for u in range(4):
                            kc = q4 * 4 + u
                            P.op("pe", lambda e: e.transpose(out=bank[:, u * 128:(u + 1) * 128], in_=t32[:, kc * 128:(kc + 1) * 128], identity=k.identf[:, :]),
                                 reads=[t32, k.identf], writes=[bank])
                        P.op("dve", lambda e: e.tensor_copy(out=hT32[:, q4 * 4:q4 * 4 + 4, :], in_=bank[:, 0:512].rearrange("p (a b) -> p a b", a=4)),
                             reads=[bank], writes=[hT32])
                    rb = k.banks[5]
                    for kc in range(16):
                        P.op("pe", lambda e: e.matmul(rb[:, 0:8], hT32[:, kc, :], rt[:, kc, :], start=(kc == 0), stop=(kc == 15)), reads=[hT32, rt], writes=[rb])
                    P.op("dve", lambda e: e.tensor_copy(out=lg[:, :], in_=rb[:, 0:8]), reads=[rb], writes=[lg])
                    P.op("dve", lambda e: e.reduce_max(out=sm[:, 0:1], in_=lg[:, :], axis=AX.X), reads=[lg], writes=[sm])
                    P.op("dve", lambda e: e.tensor_scalar(out=t8[0][:, :], in0=lg[:, :], scalar1=sm[:, 0:1], scalar2=-1e30, op0=ALU.is_ge, op1=ALU.mult),
                         reads=[lg, sm], writes=[t8[0]])
                    P.op("dve", lambda e: e.tensor_tensor(out=t8[1][:, :], in0=t8[0][:, :], in1=lg[:, :], op=ALU.add), reads=[t8[0], lg], writes=[t8[1]])
                    P.op("dve", lambda e: e.reduce_max(out=sm[:, 1:2], in_=t8[1][:, :], axis=AX.X), reads=[t8[1]], writes=[sm])
                    P.op("dve", lambda e: e.tensor_scalar(out=t8[2][:, :], in0=lg[:, :], scalar1=sm[:, 1:2], scalar2=None, op0=ALU.is_ge), reads=[lg, sm], writes=[t8[2]])
                    P.op("dve", lambda e: e.tensor_scalar_mul(out=sm[:, 2:3], in0=sm[:, 0:1], scalar1=-1.0), reads=[sm], writes=[sm])
                    P.op("act", lambda e: e.activation(out=t8[3][:, :], in_=lg[:, :], func=AF.Exp, bias=sm[:, 2:3], scale=1.0), reads=[lg, sm], writes=[t8[3]])
                    P.op("dve", lambda e: e.tensor_tensor(out=t8[0][:, :], in0=t8[3][:, :], in1=t8[2][:, :], op=ALU.mult), reads=[t8[3], t8[2]], writes=[t8[0]])
                    P.op("dve", lambda e: e.reduce_sum(out=sm[:, 3:4], in_=t8[0][:, :], axis=AX.X), reads=[t8[0]], writes=[sm])
                    P.op("dve", lambda e: e.reciprocal(out=sm[:, 3:4], in_=sm[:, 3:4]), reads=[sm], writes=[sm])
                    P.op("dve", lambda e: e.tensor_scalar_mul(out=gates[:, sub, :], in0=t8[0][:, :], scalar1=sm[:, 3:4]), reads=[t8[0], sm], writes=[gates])

            def up_chunk(wg, wu, jj, dstj):
                nonlocal cnt, nsg
                bg = k.banks[cnt % 6]
                bu = k.banks[(cnt + 1) % 6]
                cnt += 2
                for kc in range(16):
                    P.op("pe", lambda e: e.matmul(bg[:, 0:512], wg[:, kc, jj * 128:(jj + 1) * 128], hT[:, kc, :], start=(kc == 0), stop=(kc == 15)),
                         reads=[wg, hT], writes=[bg])
                for kc in range(16):
                    P.op("pe", lambda e: e.matmul(bu[:, 0:512], wu[:, kc, jj * 128:(jj + 1) * 128], hT[:, kc, :], start=(kc == 0), stop=(kc == 15)),
                         reads=[wu, hT], writes=[bu])
                sg = sg2[nsg % 2]
                nsg += 1
                P.op("act", lambda e: e.activation(out=sg[:, :], in_=bg[:, 0:512], func=AF.Silu), reads=[bg], writes=[sg])
                P.op("dve", lambda e: e.tensor_tensor(out=actT[:, dstj, :], in0=bu[:, 0:512], in1=sg[:, :], op=ALU.mult), reads=[bu, sg], writes=[actT])

            if not moe:
                for jg in range(8):
                    wg = ws.get(n)
                    wu = ws.get(n + 1)
                    n += 2
                    for jj in range(4):
                        up_chunk(wg, wu, jj, jg * 4 + jj)
                for fs in range(4):
                    w0 = ws.get(n)
                    w1 = ws.get(n + 1)
                    n += 2
                    for sub in range(4):
                        bank = k.banks[sub]
                        for j in range(32):
                            w = w0 if j < 16 else w1
                            P.op("pe", lambda e: e.matmul(bank[:, 0:512], actT[:, j, sub * 128:(sub + 1) * 128], w[:, j % 16, :], start=(j == 0), stop=(j == 31)),
                                 reads=[actT, w], writes=[bank])
                        xv = xs[:, sub, fs * 512:(fs + 1) * 512]
                        P.op("dve", lambda e: e.tensor_tensor(out=xv, in0=bank[:, 0:512], in1=xv, op=ALU.add), reads=[bank, xs], writes=[xs])
            else:
                for ex in range(8):
                    for pr in range(2):
                        wg = ws.get(n)
                        wu = ws.get(n + 1)
                        n += 2
                        for jj in range(4):
                            up_chunk(wg, wu, jj, pr * 4 + jj)
                    for fs in range(4):
                        w = ws.get(n)
                        n += 1
                        for sub in range(4):
                            bank = k.banks[cnt % 6]
                            cnt += 1
                            for j in range(8):
                                P.op("pe", lambda e: e.matmul(bank[:, 0:512], actT[:, j, sub * 128:(sub + 1) * 128], w[:, j, :], start=(j == 0), stop=(j == 7)),
                                     reads=[actT, w], writes=[bank])
                            xv = xs[:, sub, fs * 512:(fs + 1) * 512]
                            P.op("dve", lambda e: e.scalar_tensor_tensor(out=xv, in0=bank[:, 0:512], scalar=gates[:, sub, ex:ex + 1], in1=xv, op0=ALU.mult, op1=ALU.add),
                                 reads=[bank, gates, xs], writes=[xs])
            if not last:
                P.dma(rows_ap(k.XR, tt * 512, 128, 4, 0, 2048), xs[:, :, :], reads=[xs], owner=xs, load=False)
            else:
                for sub in range(4):
                    y = yst[sub % 2]
                    s2 = ss2[sub % 2]
                    r2 = rs2[sub % 2]
                    P.op("act", lambda e: e.activation(out=scr["junk"][:, :], in_=xs[:, sub, :], func=AF.Square, accum_out=s2[:, 0:1]), reads=[xs], writes=[scr["junk"], s2])
                    rsqrt_ops(k, s2, r2, 1.0 / D)
                    P.op("dve", lambda e: e.scalar_tensor_tensor(out=y[:, :], in0=xs[:, sub, :], scalar=r2[:, 1:2], in1=fn_bc[:, :], op0=ALU.mult, op1=ALU.mult),
                         reads=[xs, r2, fn_bc], writes=[y])
                    P.dma(rows_ap(k.y, tt * 512 + sub * 128, 128, 1, 0, 2048)[:, 0, :], y[:, :], reads=[y], owner=y, load=False)


def lambda_init(layer):
    return 0.8 - 0.6 * math.exp(-0.3 * layer)


def build(S, depth=4, stop_after=None, dbg=()):
    nc = bass.Bass("TRN2", target_bir_lowering=False)
    k = K()
    k.nc = nc
    k.S = S

    import os
    small = os.environ.get("MK_SMALL") == "1" and depth == 1
    SMALLSET = ("b_w_in", "b_w_out", "c_w_in", "c_w_out", "moe_w_gu", "moe_w_down")

    def din(name, shape, dt=F32):
        if small and name in SMALLSET:
            shape = [128, 8]
        return nc.dram_tensor(name, list(shape), dt, kind="ExternalInput").ap()

    def dscr(name, shape, dt):
        if name in dbg:
            return nc.dram_tensor(name, list(shape), dt, kind="ExternalOutput").ap()
        return nc.dram_tensor(name, list(shape), dt).ap()

    k.x = din("x", [S, D])
    k.c = din("c", [1, D])
    k.ada_w = din("ada_w", [4 * 2048, 12288])
    k.ada_b = din("ada_b", [4, 12288])
    k.norm_mix = din("norm_mix", [4, 2048])
    k.norm_ffn = din("norm_ffn", [4, 2048])
    k.a_w_in = din("a_w_in", [2 * 2048, 6144])
    k.a_w_out = din("a_w_out", [2 * 2048, 2048])
    k.a_lambda = din("a_lambda", [2, 512])
    k.a_subln = din("a_subln", [2, 256])
    k.b_w_in = din("b_w_in", [2048, 3072])
    k.b_w_out = din("b_w_out", [2048, 2048])
    k.b_sink = din("b_sink", [1, 16])
    k.c_w_in = din("c_w_in", [2048, 9216])
    k.c_w_out = din("c_w_out", [1024, 2048])
    k.f_w_gu = din("f_w_gu", [2 * 2048, 8192])
    k.f_w_down = din("f_w_down", [2 * 4096, 2048])
    k.moe_router = din("moe_router", [2 * 2048, 8])
    k.moe_w_gu = din("moe_w_gu", [16 * 2048, 2048])
    k.moe_w_down = din("moe_w_down", [16 * 1024, 2048])
    k.final_norm = din("final_norm", [1, 2048])
    k.rope = din("rope", [S, 32])
    identb_d = din("identb", [128, 128], BF16)
    identf_d = din("identf", [128, 128], F32)
    maskAB_d = din("maskAB", [128, 256], BF16)
    maskC_d = din("maskC", [128, 1024], BF16)
    k.y = nc.dram_tensor("y", [S, D], F32, kind="ExternalOutput").ap()

    k.XR = dscr("XR", [S, D], F32)
    k.DBG = dscr("DBG", [128, 1024], F32) if "DBG" in dbg else None
    k.MODS = dscr("MODS", [24, 2048], F32)
    QKVA = dscr("QKVA", [S, 6144], BF16)
    QKVB = dscr("QKVB", [S, 3072], BF16)
    QKVC = dscr("QKVC", [S, 9216], BF16)
    k.O = dscr("O", [S, 2048], BF16)
    k.NUMD = [dscr("NUMD%d" % i, [S, 1032], F32) for i in range(3)]
    WA_in = [dscr("WA_in%d" % i, [2048, 6144], BF16) for i in range(2)]
    WA_out = [dscr("WA_out%d" % i, [2048, 2048], BF16) for i in range(2)]
    WB_in = dscr("WB_in", [2048, 3072], BF16)
    WB_out = dscr("WB_out", [2048, 2048], BF16)
    WC_in = dscr("WC_in", [2048, 9216], BF16)
    WC_out = dscr("WC_out", [1024, 2048], BF16)
    WF_gu = [dscr("WF_gu%d" % i, [2048, 8192], BF16) for i in range(2)]
    WF_dn = [dscr("WF_dn%d" % i, [4096, 2048], BF16) for i in range(2)]
    WM_gu = dscr("WM_gu", [16 * 2048, 2048], BF16)
    WM_dn = dscr("WM_dn", [16 * 1024, 2048], BF16)

    with ExitStack() as st:
        P = Prog(nc, st)
        k.P = P
        k.banks = [P.reg(Buf("bank%d" % i, st.enter_context(nc.psum_tensor("bank%d" % i, [128, 512], F32)))) for i in range(8)]
        for b in k.banks:
            b.excl = True
        k.bview = lambda bank: bank.t[:, :].bitcast(BF16)

        def psb(name, shape, dt):
            return P.reg(Buf(name, st.enter_context(nc.sbuf_tensor(name, list(shape), dt))))

        k.identb = psb("identb_sb", [128, 128], BF16)
        k.identf = psb("identf_sb", [128, 128], F32)
        k.maskAB = psb("maskAB_sb", [128, 2, 128], BF16)
        k.maskC = psb("maskC_sb", [128, 4, 256], BF16)
        P.dma(k.identb[:, :], identb_d[:, :], writes=[k.identb], owner=k.identb)
        P.dma(k.identf[:, :], identf_d[:, :], writes=[k.identf], owner=k.identf)
        P.dma(k.maskAB[:, :, :], maskAB_d[:, :].rearrange("p (a b) -> p a b", a=2), writes=[k.maskAB], owner=k.maskAB)
        P.dma(k.maskC[:, :, :], maskC_d[:, :].rearrange("p (a b) -> p a b", a=4), writes=[k.maskC], owner=k.maskC)

        class _Stop(Exception):
            pass

        def chk(name):
            if stop_after == name:
                raise _Stop()

        try:
            _build_phases(k, depth, chk, locals())
        except _Stop:
            pass
        k.stats = (dict(P.ninstr), dict(P.nwait), len(P.dsems))
    return nc, k


def _build_phases(k, depth, chk, L):
    WA_in, WA_out, WB_in, WB_out, WC_in, WC_out = L["WA_in"], L["WA_out"], L["WB_in"], L["WB_out"], L["WC_in"], L["WC_out"]
    WF_gu, WF_dn, WM_gu, WM_dn = L["WF_gu"], L["WF_dn"], L["WM_gu"], L["WM_dn"]
    QKVA, QKVB, QKVC = L["QKVA"], L["QKVB"], L["QKVC"]
    if True:
        phase_pre(k)
        chk("pre")
        jobs = [
            (k.a_w_in[0:2048, :], WA_in[0], None), (k.a_w_out[0:2048, :], WA_out[0], 0 * 6 + 2),
            (k.f_w_gu[0:2048, :], WF_gu[0], None), (k.f_w_down[0:4096, :], WF_dn[0], 0 * 6 + 5),
        ]
        if depth > 1:
            jobs += [(k.b_w_in, WB_in, None), (k.b_w_out, WB_out, 1 * 6 + 2),
                     (k.moe_w_gu[0:16384, :], WM_gu[0:16384, :], None), (k.moe_w_down[0:8192, :], WM_dn[0:8192, :], 1 * 6 + 5)]
        if depth > 2:
            jobs += [(k.c_w_in, WC_in, None), (k.c_w_out, WC_out, 2 * 6 + 2),
                     (k.f_w_gu[2048:4096, :], WF_gu[1], None), (k.f_w_down[4096:8192, :], WF_dn[1], 2 * 6 + 5)]
        if depth > 3:
            jobs += [(k.a_w_in[2048:4096, :], WA_in[1], None), (k.a_w_out[2048:4096, :], WA_out[1], 3 * 6 + 2),
                     (k.moe_w_gu[16384:32768, :], WM_gu[16384:32768, :], None), (k.moe_w_down[8192:16384, :], WM_dn[8192:16384, :], 3 * 6 + 5)]
        phase_wconv(k, jobs)
        chk("wc")

        phase_qkv(k, 0, WA_in[0], 6144, 4096, QKVA)
        chk("p0")
        phase_att_a(k, 0, 0, QKVA, lambda_init(0))
        chk("aa0")
        phase_post(k, 0, "ab", 2048, WA_out[0], ("dense", WF_gu[0], WF_dn[0]), depth == 1)
        if depth > 1:
            phase_qkv(k, 1, WB_in, 3072, 2560, QKVB)
            phase_att_b(k, QKVB)
            phase_post(k, 1, "ab", 2048, WB_out, ("moe", 0, WM_gu, WM_dn), depth == 2)
        if depth > 2:
            phase_qkv(k, 2, WC_in, 9216, 6144, QKVC)
            phase_att_c(k, QKVC)
            phase_post(k, 2, "c", 1024, WC_out, ("dense", WF_gu[1], WF_dn[1]), depth == 3)
        if depth > 3:
            phase_qkv(k, 3, WA_in[1], 6144, 4096, QKVA)
            phase_att_a(k, 3, 1, QKVA, lambda_init(3))
            phase_post(k, 3, "ab", 2048, WA_out[1], ("moe", 1, WM_gu, WM_dn), True)


def make_consts(S):
    half = 16
    inv = (np.float32(500000.0) ** (-np.arange(half, dtype=np.float32) * np.float32(2.0 / 32))).astype(np.float32)
    ang = np.arange(S, dtype=np.float32)[:, None] * inv[None, :]
    rope = np.concatenate([np.cos(ang), np.sin(ang)], axis=1).astype(np.float32)
    bf = ml_dtypes.bfloat16
    j = np.arange(128)[:, None]
    i = np.arange(128)[None, :]
    MA = (j >= i)
    MB = (j <= i)
    MAf = MA & (j >= 64)
    MBl = MB & (j <= 63)
    maskAB = np.concatenate([MA, MB], axis=1).astype(bf)
    maskC = np.concatenate([MA, MB, MAf, MB, MA, MBl, MAf, MBl], axis=1).astype(bf)
    return {"rope": rope, "identb": np.eye(128).astype(bf), "identf": np.eye(128, dtype=np.float32),
            "maskAB": maskAB, "maskC": maskC}


def shared_inputs(inp):
    f = lambda a: np.ascontiguousarray(np.asarray(a, dtype=np.float32))
    return {
        "ada_w": f(inp["ada_w"]).reshape(4 * 2048, 12288), "ada_b": f(inp["ada_b"]),
        "norm_mix": f(inp["norm_mix"]), "norm_ffn": f(inp["norm_ffn"]),
        "a_w_in": f(inp["a_w_in"]).reshape(2 * 2048, 6144), "a_w_out": f(inp["a_w_out"]).reshape(2 * 2048, 2048),
        "a_lambda": f(inp["a_lambda"]).reshape(2, 512), "a_subln": f(inp["a_subln"]),
        "b_w_in": f(inp["b_w_in"]).reshape(2048, 3072), "b_w_out": f(inp["b_w_out"]).reshape(2048, 2048),
        "b_sink": f(inp["b_sink"]).reshape(1, 16),
        "c_w_in": f(inp["c_w_in"]).reshape(2048, 9216), "c_w_out": f(inp["c_w_out"]).reshape(1024, 2048),
        "f_w_gu": f(inp["f_w_gu"]).reshape(2 * 2048, 8192), "f_w_down": f(inp["f_w_down"]).reshape(2 * 4096, 2048),
        "moe_router": f(inp["moe_router"]).reshape(2 * 2048, 8),
        "moe_w_gu": f(inp["moe_w_gu"]).reshape(16 * 2048, 2048), "moe_w_down": f(inp["moe_w_down"]).reshape(16 * 1024, 2048),
        "final_norm": f(inp["final_norm"]).reshape(1, 2048),
    }


_CACHE = {}


def kernel(**inputs):
    xp = np.asarray(inputs["x_prompt"], dtype=np.float32)
    xsm = np.asarray(inputs["x_sample"], dtype=np.float32)
    cp = np.asarray(inputs["c_prompt"], dtype=np.float32)
    csm = np.asarray(inputs["c_sample"], dtype=np.float32)
    S = xp.shape[1]
    seqs = [(xp[b], cp[b]) for b in range(xp.shape[0])] + [(xsm[b], csm[b]) for b in range(xsm.shape[0])]
    n_cores = 8
    while len(seqs) < n_cores:
        seqs.append(seqs[len(seqs) - 6])
    if S not in _CACHE:
        _CACHE[S] = build(S)[0]
    nc = _CACHE[S]
    shared = shared_inputs(inputs)
    shared.update(make_consts(S))
    in_maps = []
    for (xs, cs) in seqs[:n_cores]:
        m = dict(shared)
        m["x"] = np.ascontiguousarray(xs)
        m["c"] = np.ascontiguousarray(cs).reshape(1, D)
        in_maps.append(m)
    res = run_bass_kernel_spmd(nc, in_maps, core_ids=list(range(n_cores)))
    ys = [np.asarray(r["y"], dtype=np.float32) for r in res.results]
    nb = xp.shape[0]
    y_prompt = np.stack(ys[0:nb], axis=0)
    y_sample = np.stack(ys[nb:nb + xsm.shape[0]], axis=0)
    return (y_prompt, y_sample)
```
